# Optimizing a Trainium2 kernel written in Bass

```python
import jax, jax.numpy as jnp
from jax import lax
import numpy as np

D_MODEL = 4096
BATCH = 2
SEQ = 4096
DEPTH = 2

EPS = 1e-6
N_MIXERS = 2
HEAD_DIM = 128
N_Q_HEADS = D_MODEL // HEAD_DIM
N_KV_HEADS = N_Q_HEADS // 4
GQA_GROUP = N_Q_HEADS // N_KV_HEADS
ROPE_THETA = 10000.0
AXIS_DIM = HEAD_DIM // 2
N_FREQ = AXIS_DIM // 2
GRID_W = 64
Q_BLOCK = 128
CHUNK = 128
GMLP_WIDTH = D_MODEL
GMLP_GROUPS = GMLP_WIDTH // 128
GMLP_GROUP_DIM = GMLP_WIDTH // GMLP_GROUPS
D_FF = 256 * ((8 * D_MODEL // 3 + 255) // 256)
N_EXPERTS = 8
TOP_K = 2
D_FF_EXPERT = D_MODEL
N_MOD = 6
MOD_SCALE = 0.3
N_ATTN = (DEPTH + 1) // 2
N_MIX = DEPTH // 2
N_DENSE = (DEPTH + 1) // 2
N_MOE = DEPTH // 2

kernel_name = "hybrid_attn_gmlp_moe_adaln_encoder"


def rmsnorm(x, g):
    xf = x.astype(jnp.float32)
    y = xf * lax.rsqrt(jnp.mean(xf * xf, axis=-1, keepdims=True) + EPS)
    return (y * g.astype(jnp.float32)).astype(x.dtype)


def modulate(x, g, shift, scale):
    return rmsnorm(x, g) * (1.0 + scale[:, None, :]) + shift[:, None, :]


def axial_rope_tables(seq):
    rows = seq // GRID_W
    row_pos = jnp.repeat(jnp.arange(rows, dtype=jnp.int32), GRID_W)
    col_pos = jnp.tile(jnp.arange(GRID_W, dtype=jnp.int32), rows)
    inv_freq = 1.0 / (ROPE_THETA ** (jnp.arange(N_FREQ, dtype=jnp.float32) * 2.0 / AXIS_DIM))
    ang = jnp.stack([row_pos.astype(jnp.float32)[:, None] * inv_freq,
                     col_pos.astype(jnp.float32)[:, None] * inv_freq], axis=1)
    return jnp.cos(ang), jnp.sin(ang)


def apply_axial_rope(x, cos, sin):
    B, S, H, _ = x.shape
    xr = x.astype(jnp.float32).reshape(B, S, H, 2, 2, N_FREQ)
    x1, x2 = xr[..., 0, :], xr[..., 1, :]
    c = cos[None, :, None]
    s = sin[None, :, None]
    out = jnp.stack([x1 * c - x2 * s, x2 * c + x1 * s], axis=-2)
    return out.reshape(B, S, H, HEAD_DIM).astype(x.dtype)


def attention_mixer(h, w_qkv, q_gain, k_gain, w_o, cos, sin):
    B, S, _ = h.shape
    qkv = h @ w_qkv
    q, k, v = jnp.split(qkv, [N_Q_HEADS * HEAD_DIM, (N_Q_HEADS + N_KV_HEADS) * HEAD_DIM], axis=-1)
    q = apply_axial_rope(rmsnorm(q.reshape(B, S, N_Q_HEADS, HEAD_DIM), q_gain), cos, sin)
    k = apply_axial_rope(rmsnorm(k.reshape(B, S, N_KV_HEADS, HEAD_DIM), k_gain), cos, sin)
    v = v.reshape(B, S, N_KV_HEADS, HEAD_DIM)
    qb = q.reshape(B, S // Q_BLOCK, Q_BLOCK, N_KV_HEADS, GQA_GROUP, HEAD_DIM)
    qb = jnp.moveaxis(qb, 1, 0)
    scale = HEAD_DIM ** -0.5

    def one_block(qi):
        s = jnp.einsum('bqkgd,bskd->bkgqs', qi, k,
                       preferred_element_type=jnp.float32) * scale
        p = jax.nn.softmax(s, axis=-1)
        return jnp.einsum('bkgqs,bskd->bqkgd', p.astype(v.dtype), v)

    o = lax.map(one_block, qb)
    o = jnp.moveaxis(o, 0, 1).reshape(B, S, N_Q_HEADS * HEAD_DIM)
    return o @ w_o


def spatial_gating_mixer(h, w_uv, v_gain, w_s, b_s, w_out):
    B, S, _ = h.shape
    z = jax.nn.gelu(h @ w_uv, approximate=False)
    u, v = jnp.split(z, 2, axis=-1)
    v = rmsnorm(v, v_gain)
    v = v.reshape(B, S // CHUNK, CHUNK, GMLP_GROUPS, GMLP_GROUP_DIM)
    sv = jnp.einsum('gpq,bnqgc->bnpgc', w_s, v) + b_s.T[None, None, :, :, None]
    return (u * sv.reshape(B, S, GMLP_WIDTH)) @ w_out


def swiglu(h, w_gate, w_up, w_down):
    return (jax.nn.silu(h @ w_gate) * (h @ w_up)) @ w_down


def moe_swiglu(h, w_router, w_gate, w_up, w_down):
    B, S, D = h.shape
    t = h.reshape(B * S, D)
    logits = (t @ w_router).astype(jnp.float32)
    top_val, top_idx = lax.top_k(logits, TOP_K)
    top_w = jax.nn.softmax(top_val, axis=-1)
    gates = jnp.sum(jax.nn.one_hot(top_idx, N_EXPERTS, dtype=jnp.float32) * top_w[..., None], axis=1)
    gates = gates.astype(t.dtype)
    out = jnp.zeros_like(t)
    for e in range(N_EXPERTS):
        y = (jax.nn.silu(t @ w_gate[e]) * (t @ w_up[e])) @ w_down[e]
        out = out + gates[:, e:e + 1] * y
    return out.reshape(B, S, D)


def setup_inputs(seed: int = 0) -> dict:
    key = jax.random.key(seed)
    ks = jax.random.split(key, 24)
    f32 = jnp.float32
    D = D_MODEL

    def nrm(k, shape, scale):
        return jax.random.normal(k, shape, f32) * scale

    def gain(k, shape):
        return 1.0 + 0.05 * jax.random.normal(k, shape, f32)

    qkv_out = (N_Q_HEADS + 2 * N_KV_HEADS) * HEAD_DIM
    return {
        "x": nrm(ks[0], (BATCH, SEQ, D), 1.0),
        "c": nrm(ks[1], (BATCH, D), 1.0),
        "w_mod": nrm(ks[2], (DEPTH, D, N_MOD * D), MOD_SCALE * D ** -0.5),
        "b_mod": nrm(ks[3], (DEPTH, N_MOD * D), 0.02),
        "norm_g": gain(ks[4], (DEPTH, 2, D)),
        "final_g": gain(ks[5], (D,)),
        "attn_w_qkv": nrm(ks[6], (N_ATTN, D, qkv_out), D ** -0.5),
        "attn_q_gain": gain(ks[7], (N_ATTN, HEAD_DIM)),
        "attn_k_gain": gain(ks[8], (N_ATTN, HEAD_DIM)),
        "attn_w_o": nrm(ks[9], (N_ATTN, N_Q_HEADS * HEAD_DIM, D), (N_Q_HEADS * HEAD_DIM) ** -0.5),
        "mix_w_uv": nrm(ks[10], (N_MIX, D, 2 * GMLP_WIDTH), D ** -0.5),
        "mix_v_gain": gain(ks[11], (N_MIX, GMLP_WIDTH)),
        "mix_w_s": nrm(ks[12], (N_MIX, GMLP_GROUPS, CHUNK, CHUNK), CHUNK ** -0.5),
        "mix_b_s": nrm(ks[13], (N_MIX, GMLP_GROUPS, CHUNK), 0.1),
        "mix_w_out": nrm(ks[14], (N_MIX, GMLP_WIDTH, D), GMLP_WIDTH ** -0.5),
        "ffn_w_gate": nrm(ks[15], (N_DENSE, D, D_FF), D ** -0.5),
        "ffn_w_up": nrm(ks[16], (N_DENSE, D, D_FF), D ** -0.5),
        "ffn_w_down": nrm(ks[17], (N_DENSE, D_FF, D), D_FF ** -0.5),
        "moe_w_router": nrm(ks[18], (N_MOE, D, N_EXPERTS), D ** -0.5),
        "moe_w_gate": nrm(ks[19], (N_MOE, N_EXPERTS, D, D_FF_EXPERT), D ** -0.5),
        "moe_w_up": nrm(ks[20], (N_MOE, N_EXPERTS, D, D_FF_EXPERT), D ** -0.5),
        "moe_w_down": nrm(ks[21], (N_MOE, N_EXPERTS, D_FF_EXPERT, D), D_FF_EXPERT ** -0.5),
    }


def reference(x, c, w_mod, b_mod, norm_g, final_g,
              attn_w_qkv, attn_q_gain, attn_k_gain, attn_w_o,
              mix_w_uv, mix_v_gain, mix_w_s, mix_b_s, mix_w_out,
              ffn_w_gate, ffn_w_up, ffn_w_down,
              moe_w_router, moe_w_gate, moe_w_up, moe_w_down):
    S = x.shape[1]
    cos, sin = axial_rope_tables(S)
    cond = jax.nn.silu(c)
    for i in range(DEPTH):
        mod = cond @ w_mod[i] + b_mod[i]
        sh1, sc1, g1, sh2, sc2, g2 = jnp.split(mod, N_MOD, axis=-1)
        j = i // N_MIXERS
        h = modulate(x, norm_g[i, 0], sh1, sc1)
        if i % N_MIXERS == 0:
            m = attention_mixer(h, attn_w_qkv[j], attn_q_gain[j], attn_k_gain[j],
                                attn_w_o[j], cos, sin)
        else:
            m = spatial_gating_mixer(h, mix_w_uv[j], mix_v_gain[j], mix_w_s[j],
                                     mix_b_s[j], mix_w_out[j])
        x = x + g1[:, None, :] * m
        h = modulate(x, norm_g[i, 1], sh2, sc2)
        if i % 2 == 0:
            f = swiglu(h, ffn_w_gate[i // 2], ffn_w_up[i // 2], ffn_w_down[i // 2])
        else:
            f = moe_swiglu(h, moe_w_router[i // 2], moe_w_gate[i // 2],
                           moe_w_up[i // 2], moe_w_down[i // 2])
        x = x + g2[:, None, :] * f
    return rmsnorm(x, final_g)
```

```python
import numpy as np
from contextlib import ExitStack
import concourse.bass as bass
import concourse.mybir as mybir
from concourse.bass_utils import run_bass_kernel_spmd

F32 = mybir.dt.float32
BF16 = mybir.dt.bfloat16
AF = mybir.ActivationFunctionType
ALU = mybir.AluOpType
AX = mybir.AxisListType
NT = 512
EPS = 1e-6


class Sem:
    def __init__(self, h, is_dma):
        self.h = h
        self.is_dma = is_dma
        self.count = 0


class Buf:
    __slots__ = ("name", "w", "r", "dsem")

    def __init__(self, name):
        self.name = name
        self.w = None
        self.r = {}
        self.dsem = None


class Eng:
    def __init__(self, name, obj, sem):
        self.name = name
        self.obj = obj
        self.sem = sem
        self.waited = {}


class K:
    def __init__(self, nc, stack):
        self.nc = nc
        self.stack = stack
        self.nsem = 0
        self.pe = Eng("pe", nc.tensor, self._sem(False))
        self.act = Eng("act", nc.scalar, self._sem(False))
        self.dve = Eng("dve", nc.vector, self._sem(False))
        self.pool = Eng("pool", nc.gpsimd, self._sem(False))
        self.sp = Eng("sp", nc.sync, self._sem(False))
        self.engs = [self.pe, self.act, self.dve, self.pool, self.sp]
        self.dsems = []

    def _sem(self, is_dma):
        self.nsem += 1
        return Sem(self.stack.enter_context(self.nc.semaphore("s%d" % self.nsem)), is_dma)

    def _wait(self, eng, deps):
        best = {}
        for s, v in deps:
            if s.is_dma:
                v = s.count
            if best.get(s, 0) < v:
                best[s] = v
        for s, v in best.items():
            if s is eng.sem and eng is self.pe:
                continue
            if eng.waited.get(s, 0) >= v:
                continue
            eng.obj.wait_ge(s.h, v)
            eng.waited[s] = v

    def _deps(self, reads, writes):
        deps = []
        for b in reads:
            if b.w is not None:
                deps.append(b.w)
        for b in writes:
            if b.w is not None:
                deps.append(b.w)
            deps.extend(b.r.items())
        return deps

    def op(self, eng, fn, reads=(), writes=()):
        self._wait(eng, self._deps(reads, writes))
        ins = fn()
        eng.sem.count += 1
        ins.then_inc(eng.sem.h, 1)
        tag = (eng.sem, eng.sem.count)
        for b in writes:
            b.w = tag
            b.r = {}
        for b in reads:
            b.r[eng.sem] = eng.sem.count
        return ins

    def dma(self, q, out, in_, reads=(), writes=(), **kw):
        self._wait(q, self._deps(reads, writes))
        b0 = writes[0]
        if b0.dsem is None:
            b0.dsem = self._sem(True)
            self.dsems.append(b0.dsem)
        s = b0.dsem
        ins = q.obj.dma_start(out=out, in_=in_, **kw)
        s.count += 16
        ins.then_inc(s.h, 16)
        tag = (s, s.count)
        for b in writes:
            b.w = tag
            b.r = {}
        for b in reads:
            b.r[s] = s.count
        return ins

    def fence(self):
        allsems = [e.sem for e in self.engs] + self.dsems
        for e in self.engs:
            self._wait(e, [(s, s.count) for s in allsems if s.count > 0 and s is not e.sem])

    def finish(self, eng):
        allsems = [e.sem for e in self.engs] + self.dsems
        self._wait(eng, [(s, s.count) for s in allsems if s.count > 0 and s is not eng.sem])


class Cfg:
    def __init__(self, D, S, NOWN, DFF, NE, DE):
        self.D, self.S, self.NOWN, self.DFF, self.NE, self.DE = D, S, NOWN, DFF, NE, DE


FULL = Cfg(4096, 4096, 1024, 11008, 8, 4096)


def build(cfg):
    D, S, NOWN, DFF, NE, DE = cfg.D, cfg.S, cfg.NOWN, cfg.DFF, cfg.NE, cfg.DE
    KC = D // 128
    NH = KC
    NKV = NH // 4
    NB = S // NT
    NP = NOWN // NT
    NQKV = (NH + 2 * NKV) * 128
    SC = S // 128
    WT = 256
    SLOT = KC * WT
    assert 2 * D == SLOT or True
    NSUB = max(1, KC // 8)
    SUBK = KC // NSUB

    nc = bass.Bass("TRN2", target_bir_lowering=False)

    def din(name, shape, dt=F32):
        return nc.dram_tensor(name, list(shape), dt, kind="ExternalInput").ap()

    xb = din("xb", [S, D])
    xo = din("xo", [NOWN, D])
    cs = din("cs", [128, S])
    sn = din("sn", [128, S])
    cso = din("cso", [128, NOWN])
    sno = din("sno", [128, NOWN])
    cT_d = din("cT", [128, KC])
    wmod = din("wmod", [2 * D, 6 * D])
    bmodT_d = din("bmodT", [128, 12 * KC])
    gT_d = din("gT", [128, 5 * KC])
    wqkv = din("wqkv", [D, NQKV])
    qkg_d = din("qkg", [128, 2])
    wo = din("wo", [D, D])
    wuv = din("wuv", [D, 2 * D])
    vgT_d = din("vgT", [128, KC])
    wsT_d = din("wsT", [128, KC * 128])
    bsb_d = din("bsb", [128, KC * 128])
    wout = din("wout", [D, D])
    wg = din("wg", [D, DFF])
    wu = din("wu", [D, DFF])
    wd = din("wd", [DFF, D])
    wr_d = din("wr", [128, KC * NE])
    mg = din("mg", [NE * D, DE])
    mu = din("mu", [NE * D, DE])
    md = din("md", [NE * DE, D])
    ident_d = din("ident", [128, 128])
    rot_d = din("rot", [128, 128])
    y = nc.dram_tensor("y", [NOWN, D], F32, kind="ExternalOutput").ap()
    Kscr = nc.dram_tensor("Kscr", [NKV, 128, S], BF16).ap()
    Vscr = nc.dram_tensor("Vscr", [NKV, SC, 128, 128], BF16).ap()

    with ExitStack() as st:
        k = K(nc, st)

        uniq = [0]

        def sb(stack, name, shape, dt):
            uniq[0] += 1
            return stack.enter_context(nc.sbuf_tensor("%s_%d" % (name, uniq[0]), list(shape), dt))

        def A(fn, r=(), w=()):
            return k.op(k.act, fn, r, w)

        def V(fn, r=(), w=()):
            return k.op(k.dve, fn, r, w)

        def P(fn, r=(), w=()):
            return k.op(k.pe, fn, r, w)

        xT = sb(st, "xT", [128, KC, NT], F32)
        hT = sb(st, "hT", [128, KC * NT], BF16)
        hT3 = hT[:, :].rearrange("p (a b) -> p a b", a=KC)
        ident = sb(st, "ident", [128, 128], F32)
        identb = sb(st, "identb", [128, 128], BF16)
        rot = sb(st, "rot", [128, 128], F32)
        ones = sb(st, "ones", [128, 128], F32)
        onesb = sb(st, "onesb", [128, 128], BF16)
        epsb = sb(st, "epsb", [128, 1], F32)
        cT = sb(st, "cTs", [128, KC], F32)
        cond = sb(st, "cond", [128, KC], BF16)
        bmodT = sb(st, "bmodTs", [128, 12 * KC], F32)
        modT = sb(st, "modT", [128, 12 * KC], F32)
        gT = sb(st, "gTs", [128, 5 * KC], F32)
        gsc = sb(st, "gsc", [128, 4 * KC], F32)
        qkg = sb(st, "qkgs", [128, 2], F32)
        vgT = sb(st, "vgTs", [128, KC], F32)
        wr = sb(st, "wrs", [128, KC * NE], F32)
        rstd = sb(st, "rstd", [128, NT], F32)
        tmpf = [sb(st, "tmpf%d" % i, [128, NT], F32) for i in range(4)]
        sqb = [sb(st, "sqb%d" % i, [128, NT], BF16) for i in range(2)]
        SQB = [Buf("sqb0"), Buf("sqb1")]
        ps = st.enter_context(nc.psum_tensor("ps", [128, 7, 512], F32))
        psb = st.enter_context(nc.psum_tensor("psb", [128, 1024], BF16))

        XB = [Buf("x%d" % i) for i in range(KC)]
        HT = Buf("hT")
        PB = [Buf("pb%d" % i) for i in range(7)]
        PBB = Buf("pbb")
        CONST = Buf("const")
        CMISC = Buf("cmisc")
        MODB = Buf("mod")
        RSTD = Buf("rstd")
        TB = [Buf("tmpf%d" % i) for i in range(4)]
        KS = Buf("Kscr")
        VS = Buf("Vscr")
        YB = Buf("y")
        RING = [Buf("ring%d" % i) for i in range(6)]

        for dst, src in [(ident, ident_d), (rot, rot_d), (cT, cT_d), (bmodT, bmodT_d), (gT, gT_d),
                         (qkg, qkg_d), (vgT, vgT_d), (wr, wr_d)]:
            k.dma(k.sp, dst[:], src, writes=[CONST])
        V(lambda: nc.vector.memset(ones[:], 1.0), w=[CMISC])
        V(lambda: nc.vector.memset(onesb[:], 1.0), w=[CMISC])
        V(lambda: nc.vector.memset(epsb[:], EPS), w=[CMISC])
        V(lambda: nc.vector.tensor_copy(out=identb[:], in_=ident[:]), r=[CONST], w=[CMISC])
        A(lambda: nc.scalar.activation(out=cond[:], in_=cT[:], func=AF.Silu), r=[CONST], w=[CMISC])
        k.fence()

        class Stream:
            def __init__(self, tiles, bufs, srcs, live=1):
                self.tiles, self.bufs, self.srcs, self.live = tiles, bufs, srcs, live
                self.issued = 0

            def get(self, i):
                n = len(self.tiles)
                upto = max(i, min(i + n - self.live, len(self.srcs) - 1))
                while self.issued <= upto:
                    j = self.issued
                    src, shape3 = self.srcs[j]
                    t = self.tiles[j % n]
                    dst = t[:, 0:shape3[0] * shape3[1]].rearrange("p (a b) -> p a b", a=shape3[0])
                    if shape3[1] * 4 > 4096:
                        for c_ in range(shape3[0]):
                            k.dma(k.pool, dst[:, c_, :], src[:, c_, :], writes=[self.bufs[j % n]], max_dma_last_dim=4096)
                    else:
                        k.dma(k.pool, dst, src, writes=[self.bufs[j % n]], max_dma_last_dim=4096)
                    self.issued += 1
                t = self.tiles[i % n]
                shape3 = self.srcs[i][1]
                return t[:, 0:shape3[0] * shape3[1]].rearrange("p (a b) -> p a b", a=shape3[0]), self.bufs[i % n]

        def wsrc(w, r0, c0, ncols=WT):
            return (w[r0:r0 + D, :].rearrange("(kc p) n -> p kc n", p=128)[:, :, c0:c0 + ncols], (KC, ncols))

        def dsrc(w, r0):
            return (w[r0:r0 + 256, :].rearrange("(c p) n -> p c n", p=128), (2, D))

        def mm_group(bank_ap, lhs_fn, rhs_fn, nk, reads, bankbuf, pieces=None):
            nsub = max(1, nk // SUBK) if nk >= SUBK else 1
            per = nk // nsub
            for s_ in range(nsub):
                def body(s_=s_):
                    ins = None
                    for kk in range(s_ * per, (s_ + 1) * per):
                        ins = nc.tensor.matmul(bank_ap, lhs_fn(kk), rhs_fn(kk), start=(kk == 0), stop=(kk == nk - 1))
                    return ins
                P(body, r=reads, w=[bankbuf])
                if pieces is not None:
                    pieces()

        MB = [[Buf("mod%d_%d" % (li, j)) for j in range(6)] for li in range(2)]
        GSCB = [Buf("gsc%d" % i) for i in range(4)]

        def emit_gsc(s_):
            li, sub = s_ // 2, s_ % 2
            sc = modT[:, li * 6 * KC + (sub * 3 + 1) * KC: li * 6 * KC + (sub * 3 + 2) * KC]
            V(lambda: nc.vector.scalar_tensor_tensor(out=gsc[:, s_ * KC:(s_ + 1) * KC], in0=sc, scalar=1.0,
                                                     in1=gT[:, s_ * KC:(s_ + 1) * KC], op0=ALU.add, op1=ALU.mult),
              r=[MB[li][sub * 3 + 1], CONST], w=[GSCB[s_]])

        def modcol(li, j, kc):
            c = li * 6 * KC + j * KC + kc
            return modT[:, c:c + 1]

        XT = [Buf("xt0"), Buf("xt1")]

        def load_xT(src_rows, ph, xt=None):
            HW = D // 2
            if xt is None:
                xt = [sb(ph, "xt%d" % i, [128, HW], F32) for i in range(2)]
            cnt = 0
            hcnt_ = 0
            for tt in range(NT // 128):
                for hf in range(2):
                    xi = hcnt_ % 2
                    hcnt_ += 1
                    k.dma(k.sp, xt[xi][:], src_rows[tt * 128:(tt + 1) * 128, hf * HW:(hf + 1) * HW], writes=[XT[xi]])
                    for k4 in range(hf * (KC // 8), (hf + 1) * (KC // 8)):
                        bank = 2 + (cnt % 4)

                        def body(tt=tt, k4=k4, bank=bank, xi=xi, hf=hf):
                            ins = None
                            for i in range(4):
                                kc = k4 * 4 + i
                                lo = kc * 128 - hf * HW
                                ins = nc.tensor.transpose(ps[:, bank, i * 128:(i + 1) * 128], xt[xi][:, lo:lo + 128], ident[:])
                            return ins
                        P(body, r=[XT[xi], CONST], w=[PB[bank]])
                        dst = xT[:, k4 * 4:(k4 + 1) * 4, tt * 128:(tt + 1) * 128]
                        src = ps[:, bank, :].rearrange("p (a b) -> p a b", a=4)
                        wb = [XB[k4 * 4 + i] for i in range(4)]
                        if cnt % 2 == 0:
                            A(lambda dst=dst, src=src: nc.scalar.copy(out=dst, in_=src), r=[PB[bank]], w=wb)
                        else:
                            V(lambda dst=dst, src=src: nc.vector.tensor_copy(out=dst, in_=src), r=[PB[bank]], w=wb)
                        cnt += 1

        def compute_rstd(nfeat_inv):
            for kc in range(KC):
                t = kc % 2
                A(lambda kc=kc, t=t: nc.scalar.activation(out=sqb[t][:], in_=xT[:, kc, :], func=AF.Square), r=[XB[kc]], w=[SQB[t]])
                P(lambda kc=kc, t=t: nc.tensor.matmul(ps[:, 0, :], onesb[:], sqb[t][:], start=(kc == 0), stop=(kc == KC - 1)),
                  r=[SQB[t], CMISC], w=[PB[0]])
            A(lambda: nc.scalar.activation(out=rstd[:], in_=ps[:, 0, :], func=AF.Ln, scale=nfeat_inv, bias=epsb[:]), r=[PB[0], CMISC], w=[RSTD])
            A(lambda: nc.scalar.activation(out=rstd[:], in_=rstd[:], func=AF.Exp, scale=-0.5), r=[RSTD], w=[RSTD])

        def norm_mod(s_, router=False, rt=None, RT=None):
            li, sub = s_ // 2, s_ % 2
            compute_rstd(1.0 / D)
            for kc in range(KC):
                t = 2 + (kc % 2)
                V(lambda kc=kc, t=t: nc.vector.scalar_tensor_tensor(out=tmpf[t][:], in0=xT[:, kc, :], scalar=gsc[:, s_ * KC + kc:s_ * KC + kc + 1],
                                                                    in1=rstd[:], op0=ALU.mult, op1=ALU.mult),
                  r=[XB[kc], RSTD, GSCB[s_]], w=[TB[t]])
                shc = modcol(li, sub * 3, kc)
                if not router:
                    A(lambda kc=kc, t=t, shc=shc: nc.scalar.activation(out=hT3[:, kc, :], in_=tmpf[t][:], func=AF.Identity, bias=shc, scale=1.0),
                      r=[TB[t], MB[li][sub * 3]], w=[HT])
                else:
                    t2 = kc % 2
                    A(lambda kc=kc, t=t, t2=t2, shc=shc: nc.scalar.activation(out=rt[t2][:], in_=tmpf[t][:], func=AF.Identity, bias=shc, scale=1.0),
                      r=[TB[t], MB[li][sub * 3]], w=[RT[t2]])
                    V(lambda kc=kc, t2=t2: nc.vector.tensor_copy(out=hT3[:, kc, :], in_=rt[t2][:]), r=[RT[t2]], w=[HT])
                    P(lambda kc=kc, t2=t2: nc.tensor.matmul(ps[0:NE, 1, :], wr[:, kc * NE:(kc + 1) * NE], rt[t2][:], start=(kc == 0), stop=(kc == KC - 1)),
                      r=[RT[t2], CONST], w=[PB[1]])

        def qk_epilogue(bank, gain_ap, cos_ap, sin_ap, dst_ap, rbufs, wbufs, t4, T4):
            A(lambda: nc.scalar.activation(out=tmpf[0][:], in_=ps[:, bank, :], func=AF.Square), r=[PB[bank]], w=[TB[0]])
            P(lambda: nc.tensor.matmul(ps[:, 0, :], ones[:], tmpf[0][:], start=True, stop=True), r=[TB[0], CMISC], w=[PB[0]])
            A(lambda: nc.scalar.activation(out=tmpf[1][:], in_=ps[:, 0, :], func=AF.Ln, scale=1.0 / 128, bias=epsb[:]), r=[PB[0], CMISC], w=[TB[1]])
            A(lambda: nc.scalar.activation(out=tmpf[1][:], in_=tmpf[1][:], func=AF.Exp, scale=-0.5), r=[TB[1]], w=[TB[1]])
            V(lambda: nc.vector.scalar_tensor_tensor(out=tmpf[2][:], in0=ps[:, bank, :], scalar=gain_ap, in1=tmpf[1][:], op0=ALU.mult, op1=ALU.mult),
              r=[PB[bank], TB[1], CONST], w=[TB[2]])
            P(lambda: nc.tensor.matmul(ps[:, 1, :], rot[:], tmpf[2][:], start=True, stop=True), r=[TB[2], CONST], w=[PB[1]])
            V(lambda: nc.vector.tensor_tensor(out=tmpf[3][:], in0=tmpf[2][:], in1=cos_ap, op=ALU.mult), r=[TB[2]] + rbufs, w=[TB[3]])
            V(lambda: nc.vector.tensor_tensor(out=t4[:], in0=ps[:, 1, :], in1=sin_ap, op=ALU.mult), r=[PB[1]] + rbufs, w=[T4])
            V(lambda: nc.vector.tensor_tensor(out=dst_ap, in0=tmpf[3][:], in1=t4[:], op=ALU.add), r=[TB[3], T4], w=wbufs)

        def proj(stm, t0, nchunks, rhs_fn, rbufs, epilogue, banks, cpt=2):
            for oc in range(nchunks):
                tl, tb = stm.get(t0 + oc // cpt)
                c2 = oc % cpt
                bank = banks[oc % len(banks)]
                mm_group(ps[:, bank, :], lambda kc, tl=tl, c2=c2: tl[:, kc, c2 * 128:(c2 + 1) * 128], rhs_fn, KC, [tb] + rbufs, PB[bank])
                epilogue(oc, bank)

        KQ = min(8, KC)
        NQ = KC // KQ

        def wsrc4(w, r0, c0, q, ncols):
            return (w[r0 + q * KQ * 128:r0 + (q + 1) * KQ * 128, :].rearrange("(kc p) n -> p kc n", p=128)[:, :, c0:c0 + ncols], (KQ, ncols))

        def srcs4(w, r0, c0, nchunks):
            gw = min(4, nchunks)
            out = []
            for grp in range(nchunks // gw):
                for q in range(NQ):
                    out.append(wsrc4(w, r0, c0 + grp * gw * 128, q, gw * 128))
            return out

        def proj4(stm, t0, nchunks, rhs_fn, rbufs, epilogue, banks4):
            gw = min(4, nchunks)
            for grp in range(nchunks // gw):
                for q in range(NQ):
                    tl, tb = stm.get(t0 + grp * NQ + q)
                    for c in range(gw):
                        bank = banks4[c]

                        def body(tl=tl, c=c, q=q, bank=bank):
                            ins = None
                            for kk in range(KQ):
                                kc = q * KQ + kk
                                ins = nc.tensor.matmul(ps[:, bank, :], tl[:, kk, c * 128:(c + 1) * 128], rhs_fn(kc), start=(kc == 0), stop=(kc == KC - 1))
                            return ins
                        P(body, r=[tb] + rbufs, w=[PB[bank]])
                        if q == NQ - 1:
                            epilogue(grp * gw + c, bank)

        def resid_epilogue(gj, li):
            def ep(oc, bank):
                V(lambda: nc.vector.scalar_tensor_tensor(out=xT[:, oc, :], in0=ps[:, bank, :], scalar=modcol(li, gj, oc), in1=xT[:, oc, :],
                                                         op0=ALU.mult, op1=ALU.add),
                  r=[PB[bank], MB[li][gj], XB[oc]], w=[XB[oc]])
            return ep

        with ExitStack() as ph:
            tiles = [sb(ph, "rk%d" % i, [128, KQ * 512], BF16) for i in range(3)]
            mt = [sb(ph, "mt%d" % i, [128, KQ * 512], BF16) for i in range(4)]
            MRING = [Buf("mring%d" % i) for i in range(4)]
            rowt = sb(ph, "rowt", [1, 512], F32)
            ROWT = Buf("rowt")
            NCG = 6 * D // 512
            units = [(0, cg) for cg in range(NCG)] + [(1, cg) for cg in range(NCG)]
            msrcs = []
            for (li_, cg_) in units:
                for q in range(NQ):
                    msrcs.append(wsrc4(wmod, li_ * D, cg_ * 512, q, 512))
            mstm = Stream(mt, MRING, msrcs)
            ucnt = [0]

            def mod_unit():
                u = ucnt[0]
                if u >= len(units):
                    return
                ucnt[0] += 1
                li_, cg_ = units[u]
                for q in range(NQ):
                    tl, tb = mstm.get(u * NQ + q)

                    def body(tl=tl, q=q):
                        ins = None
                        for kk in range(KQ):
                            kc = q * KQ + kk
                            ins = nc.tensor.matmul(ps[0:1, 6, :], cond[:, kc:kc + 1], tl[:, kk, :], start=(kc == 0), stop=(kc == KC - 1))
                        return ins
                    P(body, r=[tb, CMISC], w=[PB[6]])
                A(lambda: nc.scalar.copy(out=rowt[0:1, :], in_=ps[0:1, 6, :]), r=[PB[6]], w=[ROWT])

                def body2():
                    ins = None
                    for c in range(4):
                        ins = nc.tensor.matmul(ps[:, 1, c:c + 1], rowt[0:1, c * 128:(c + 1) * 128], ones[0:1, 0:1], start=True, stop=True)
                    return ins
                P(body2, r=[ROWT, CMISC], w=[PB[1]])
                c0 = li_ * 6 * KC + cg_ * 4
                j_ = (cg_ * 4) // KC
                V(lambda: nc.vector.tensor_tensor(out=modT[:, c0:c0 + 4], in0=ps[:, 1, 0:4], in1=bmodT[:, c0:c0 + 4], op=ALU.add),
                  r=[PB[1], CONST], w=[MB[li_][j_]])
            for _ in range(2 * KC // 4):
                mod_unit()
            emit_gsc(0)
            csb = [sb(ph, "csb%d" % i, [128, NT], F32) for i in range(2)]
            snb = [sb(ph, "snb%d" % i, [128, NT], F32) for i in range(2)]
            CS = [Buf("cs0"), Buf("cs1")]
            kst = [sb(ph, "kst%d" % i, [128, NT], BF16) for i in range(2)]
            KST = [Buf("kst0"), Buf("kst1")]
            vfm = [sb(ph, "vfm%d" % i, [128, NT], BF16) for i in range(2)]
            VFM = [Buf("vfm0"), Buf("vfm1")]
            vtk = [sb(ph, "vtk%d" % i, [128, NT], BF16) for i in range(2)]
            VTK = [Buf("vtk0"), Buf("vtk1")]
            t4k = sb(ph, "t4k", [128, NT], F32)
            T4K = Buf("t4k")
            xt_kv = [sb(ph, "xtkv%d" % i, [128, D // 2], F32) for i in range(2)]
            for blk in range(NB):
                load_xT(xb[blk * NT:(blk + 1) * NT, :], ph, xt_kv)
                norm_mod(0)
                cb = blk % 2
                k.dma(k.sp, csb[cb][:], cs[:, blk * NT:(blk + 1) * NT], writes=[CS[cb]])
                k.dma(k.sp, snb[cb][:], sn[:, blk * NT:(blk + 1) * NT], writes=[CS[cb]])
                stm = Stream(tiles, RING[:3], srcs4(wqkv, 0, NH * 128, NKV))
                cntk = [0]

                def k_ep(oc, bank, blk=blk, cb=cb):
                    i = cntk[0] % 2
                    cntk[0] += 1
                    qk_epilogue(bank, qkg[:, 1:2], csb[cb][:], snb[cb][:], kst[i][:], [CS[cb]], [KST[i]], t4k, T4K)
                    k.dma(k.sp, Kscr[oc, :, blk * NT:(blk + 1) * NT], kst[i][:], reads=[KST[i]], writes=[KS])
                    mod_unit()
                proj4(stm, 0, NKV, lambda kc: hT3[:, kc, :], [HT], k_ep, [2, 3, 4, 5])
                cntv = [0]

                def v_ep(oc, bank, blk=blk):
                    i = cntv[0] % 2
                    cntv[0] += 1
                    A(lambda: nc.scalar.copy(out=vfm[i][:], in_=ps[:, bank, :]), r=[PB[bank]], w=[VFM[i]])

                    def body():
                        ins = None
                        for c in range(NT // 128):
                            ins = nc.tensor.transpose(psb[:, c * 128:(c + 1) * 128], vfm[i][:, c * 128:(c + 1) * 128], identb[:])
                        return ins
                    P(body, r=[VFM[i], CMISC], w=[PBB])
                    V(lambda: nc.vector.tensor_copy(out=vtk[i][:], in_=psb[:, 0:NT]), r=[PBB], w=[VTK[i]])
                    k.dma(k.sp, Vscr[oc, blk * (NT // 128):(blk + 1) * (NT // 128)].rearrange("c p d -> p c d"),
                          vtk[i][:, :].rearrange("p (c d) -> p c d", d=128), reads=[VTK[i]], writes=[VS])
                    mod_unit()
                stm = Stream(tiles, RING[:3], srcs4(wqkv, 0, (NH + NKV) * 128, NKV))
                proj4(stm, 0, NKV, lambda kc: hT3[:, kc, :], [HT], v_ep, [2, 3, 4, 5])
            while ucnt[0] < len(units):
                mod_unit()
            for s_ in range(1, 4):
                emit_gsc(s_)
            k.fence()

        def glu(groups, li, ph):
            NG = len(groups)
            tiles = [sb(ph, "rg%d" % i, [128, SLOT], BF16) for i in range(5)]
            acts = [sb(ph, "act%d" % i, [128, NT], BF16) for i in range(4)]
            ACTB = [Buf("act%d" % i) for i in range(4)]
            srcs = []
            tidx = {}
            for g in range(NG + 1):
                if g < NG:
                    tidx[("g", g)] = len(srcs)
                    srcs.append(groups[g][0])
                    tidx[("u", g)] = len(srcs)
                    srcs.append(groups[g][1])
                if g >= 1:
                    tidx[("d", g - 1)] = len(srcs)
                    srcs.append(groups[g - 1][2])
            stm = Stream(tiles, RING[:5], srcs, live=3)
            dcnt = [0]

            def d_piece(g, dc, dtl, dtb):
                bank = 4 + (dcnt[0] % 3)
                dcnt[0] += 1
                a0, a1 = (2 * g) % 4, (2 * g + 1) % 4

                def body():
                    nc.tensor.matmul(ps[:, bank, :], dtl[:, 0, dc * 128:(dc + 1) * 128], acts[a0][:], start=True, stop=False)
                    return nc.tensor.matmul(ps[:, bank, :], dtl[:, 1, dc * 128:(dc + 1) * 128], acts[a1][:], start=False, stop=True)
                P(body, r=[dtb, ACTB[a0], ACTB[a1]], w=[PB[bank]])
                resid_epilogue(5, li)(dc, bank)

            for g in range(NG + 1):
                pending = []
                if g >= 1:
                    pending = list(range(KC))
                dstate = {}

                def pieces(g=g, pending=pending, dstate=dstate, n=None):
                    if not pending:
                        return
                    if "t" not in dstate:
                        return
                    cnt = n if n is not None else max(1, -(-KC // (4 * NSUB)))
                    for _ in range(cnt):
                        if pending:
                            d_piece(g - 1, pending.pop(0), dstate["t"][0], dstate["t"][1])
                if g < NG:
                    gtl, gtb = stm.get(tidx[("g", g)])
                    utl, utb = stm.get(tidx[("u", g)])
                    if g >= 1:
                        dstate["t"] = stm.get(tidx[("d", g - 1)])
                    mul_ap, mul_bufs = groups[g][3], groups[g][4]
                    if len(groups[g]) > 5 and groups[g][5] is not None:
                        groups[g][5]()
                    for c2 in range(2):
                        bg, bu = c2 * 2, c2 * 2 + 1
                        mm_group(ps[:, bg, :], lambda kc, c2=c2: gtl[:, kc, c2 * 128:(c2 + 1) * 128], lambda kc: hT3[:, kc, :], KC, [gtb, HT], PB[bg], pieces)
                        mm_group(ps[:, bu, :], lambda kc, c2=c2: utl[:, kc, c2 * 128:(c2 + 1) * 128], lambda kc: hT3[:, kc, :], KC, [utb, HT], PB[bu], pieces)
                        t = c2
                        ai = (2 * g + c2) % 4
                        A(lambda bg=bg, t=t: nc.scalar.activation(out=tmpf[t][:], in_=ps[:, bg, :], func=AF.Silu), r=[PB[bg]], w=[TB[t]])
                        if mul_ap is not None:
                            V(lambda t=t: nc.vector.tensor_tensor(out=tmpf[t][:], in0=tmpf[t][:], in1=mul_ap, op=ALU.mult), r=[TB[t]] + mul_bufs, w=[TB[t]])
                        V(lambda bu=bu, t=t, ai=ai: nc.vector.tensor_tensor(out=acts[ai][:], in0=tmpf[t][:], in1=ps[:, bu, :], op=ALU.mult),
                          r=[TB[t], PB[bu]], w=[ACTB[ai]])
                    pieces(n=KC)
                else:
                    dstate["t"] = stm.get(tidx[("d", g - 1)])
                    pieces(n=KC)

        for pz in range(NP):
            tok0 = pz * NT
            with ExitStack() as ph:
                load_xT(xo[tok0:tok0 + NT, :], ph)
            k.fence()
            norm_mod(0)
            with ExitStack() as ph:
                QT = sb(ph, "QT", [128, NH, NT], BF16)
                QB = [Buf("q%d" % h) for h in range(NH)]
                with ExitStack() as ph2:
                    tiles = [sb(ph2, "rq%d" % i, [128, KQ * 512], BF16) for i in range(4)]
                    cso_t = sb(ph2, "cso_t", [128, NT], F32)
                    sno_t = sb(ph2, "sno_t", [128, NT], F32)
                    CSO = Buf("cso")
                    t4q = sb(ph2, "t4q", [128, NT], F32)
                    T4Q = Buf("t4q")
                    k.dma(k.sp, cso_t[:], cso[:, tok0:tok0 + NT], writes=[CSO])
                    k.dma(k.sp, sno_t[:], sno[:, tok0:tok0 + NT], writes=[CSO])
                    stm = Stream(tiles, RING[:4], srcs4(wqkv, 0, 0, NH))

                    def q_ep(oc, bank):
                        qk_epilogue(bank, qkg[:, 0:1], cso_t[:], sno_t[:], QT[:, oc, :], [CSO], [QB[oc]], t4q, T4Q)
                    proj4(stm, 0, NH, lambda kc: hT3[:, kc, :], [HT], q_ep, [2, 3, 4, 5])
                    k.fence()
                with ExitStack() as ph2:
                    kT = [sb(ph2, "kT%d" % i, [128, S], BF16) for i in range(2)]
                    vt = [sb(ph2, "vt%d" % i, [128, SC, 128], BF16) for i in range(2)]
                    KTB = [Buf("kT0"), Buf("kT1")]
                    VTB = [Buf("vt0"), Buf("vt1")]
                    pT = [sb(ph2, "pT%d" % i, [128, NT], BF16) for i in range(3)]
                    PTB = [Buf("pT%d" % i) for i in range(3)]
                    rl = [sb(ph2, "rl%d" % i, [128, NT], F32) for i in range(2)]
                    RL = [Buf("rl0"), Buf("rl1")]
                    scale = 128.0 ** -0.5
                    hcnt = 0
                    for j in range(NKV):
                        jb = j % 2
                        k.dma(k.sp, kT[jb][:], Kscr[j], reads=[KS], writes=[KTB[jb]])
                        k.dma(k.sp, vt[jb][:], Vscr[j].rearrange("c p d -> p c d"), reads=[VS], writes=[VTB[jb]])
                        for i4 in range(4):
                            h = j * 4 + i4
                            bo, bl = (3, 4) if hcnt % 2 == 0 else (5, 6)
                            hcnt += 1

                            def qk(c, h=h, jb=jb):
                                sbk = 1 + (c % 2)
                                P(lambda: nc.tensor.matmul(ps[:, sbk, :], kT[jb][:, c * 128:(c + 1) * 128], QT[:, h, :], start=True, stop=True),
                                  r=[KTB[jb], QB[h]], w=[PB[sbk]])
                            qk(0)
                            for c in range(SC):
                                if c + 1 < SC:
                                    qk(c + 1)
                                sbk = 1 + (c % 2)
                                pi = c % 3
                                A(lambda sbk=sbk, pi=pi: nc.scalar.activation(out=pT[pi][:], in_=ps[:, sbk, :], func=AF.Exp, scale=scale),
                                  r=[PB[sbk]], w=[PTB[pi]])

                                def body(c=c, pi=pi, jb=jb, bo=bo, bl=bl):
                                    nc.tensor.matmul(ps[:, bo, :], vt[jb][:, c, :], pT[pi][:], start=(c == 0), stop=(c == SC - 1))
                                    return nc.tensor.matmul(ps[:, bl, :], onesb[:], pT[pi][:], start=(c == 0), stop=(c == SC - 1))
                                P(body, r=[VTB[jb], PTB[pi], CMISC], w=[PB[bo], PB[bl]])
                            t = hcnt % 2
                            A(lambda bl=bl, t=t: nc.scalar.activation(out=rl[t][:], in_=ps[:, bl, :], func=AF.Ln), r=[PB[bl]], w=[RL[t]])
                            A(lambda t=t: nc.scalar.activation(out=rl[t][:], in_=rl[t][:], func=AF.Exp, scale=-1.0), r=[RL[t]], w=[RL[t]])
                            V(lambda bo=bo, t=t, h=h: nc.vector.tensor_tensor(out=QT[:, h, :], in0=ps[:, bo, :], in1=rl[t][:], op=ALU.mult),
                              r=[PB[bo], RL[t]], w=[QB[h]])
                    k.fence()
                with ExitStack() as ph2:
                    tiles = [sb(ph2, "ro%d" % i, [128, KQ * 512], BF16) for i in range(4)]
                    stm = Stream(tiles, RING[:4], srcs4(wo, 0, 0, KC))
                    proj4(stm, 0, KC, lambda kc: QT[:, kc, :], QB, resid_epilogue(2, 0), [1, 2, 3, 4])
                    k.fence()
            norm_mod(1)
            with ExitStack() as ph:
                groups = [(wsrc(wg, 0, g * WT), wsrc(wu, 0, g * WT), dsrc(wd, g * 256), None, []) for g in range(DFF // 256)]
                glu(groups, 0, ph)
                k.fence()
            norm_mod(2)
            with ExitStack() as ph:
                vgTt = sb(ph, "vgTt", [128, KC, NT], BF16)
                uT = sb(ph, "uT", [128, KC, NT], BF16)
                sqacc = tmpf[3]
                rsk = sb(ph, "rsk", [128, 4], F32)
                vtok = sb(ph, "vtok", [128, NT], BF16)
                svt = tmpf[2]
                VG = Buf("vg")
                UB = [Buf("u%d" % g) for g in range(KC)]
                SQ = TB[3]
                RSK = Buf("rsk")
                VTOK = Buf("vtok")
                SVT = TB[2]
                tiles = [sb(ph, "rx%d" % i, [128, KQ * 512], BF16) for i in range(3)]
                srcs = srcs4(wuv, 0, D, KC) + srcs4(wuv, 0, 0, KC)
                stm = Stream(tiles, RING[:3], srcs)

                def v_ep2(oc, bank):
                    A(lambda: nc.scalar.activation(out=tmpf[0][:], in_=ps[:, bank, :], func=AF.Gelu), r=[PB[bank]], w=[TB[0]])
                    A(lambda: nc.scalar.activation(out=tmpf[1][:], in_=tmpf[0][:], func=AF.Square), r=[TB[0]], w=[TB[1]])
                    if oc == 0:
                        V(lambda: nc.vector.tensor_copy(out=sqacc[:], in_=tmpf[1][:]), r=[TB[1]], w=[SQ])
                    else:
                        V(lambda: nc.vector.tensor_tensor(out=sqacc[:], in0=sqacc[:], in1=tmpf[1][:], op=ALU.add), r=[TB[1], SQ], w=[SQ])
                    V(lambda: nc.vector.tensor_scalar(out=vgTt[:, oc, :], in0=tmpf[0][:], scalar1=vgT[:, oc:oc + 1], scalar2=None, op0=ALU.mult),
                      r=[TB[0], CONST], w=[VG])
                proj4(stm, 0, KC, lambda kc: hT3[:, kc, :], [HT], v_ep2, [2, 3, 4, 5])
                for n in range(NT // 128):
                    P(lambda n=n: nc.tensor.matmul(ps[:, 1, n:n + 1], sqacc[:, n * 128:(n + 1) * 128], ones[:, 0:1], start=True, stop=True),
                      r=[SQ, CMISC], w=[PB[1]])
                A(lambda: nc.scalar.activation(out=rsk[:], in_=ps[:, 1, 0:4], func=AF.Sqrt, scale=1.0 / D, bias=epsb[:]), r=[PB[1], CMISC], w=[RSK])
                V(lambda: nc.vector.reciprocal(out=rsk[:], in_=rsk[:]), r=[RSK], w=[RSK])
                def u_ep(oc, bank):
                    A(lambda: nc.scalar.activation(out=uT[:, oc, :], in_=ps[:, bank, :], func=AF.Gelu), r=[PB[bank]], w=[UB[oc]])
                proj4(stm, (KC // min(4, KC)) * NQ, KC, lambda kc: hT3[:, kc, :], [HT], u_ep, [2, 3, 4, 5])
                k.fence()
                bias_bc = hT[:, 0:KC * 256].bitcast(F32)
                wsTb = hT[:, KC * 256:KC * 384]
                wsS = hT[:, KC * 384:KC * 512]
                WSS = Buf("wsS")
                k.dma(k.sp, bias_bc, bsb_d, writes=[HT])
                k.dma(k.pool, wsTb, wsT_d, writes=[HT], max_dma_last_dim=4096)
                for n in range(NT // 128):
                    V(lambda n=n: nc.vector.tensor_scalar(out=wsS, in0=wsTb, scalar1=rsk[:, n:n + 1], scalar2=None, op0=ALU.mult),
                      r=[HT, RSK], w=[WSS])
                    for g4 in range(KC // 4):
                        def body(n=n, g4=g4):
                            ins = None
                            for i in range(4):
                                ins = nc.tensor.transpose(psb[:, i * 128:(i + 1) * 128], vgTt[:, g4 * 4 + i, n * 128:(n + 1) * 128], identb[:])
                            return ins
                        P(body, r=[VG, CMISC], w=[PBB])
                        A(lambda: nc.scalar.copy(out=vtok[:], in_=psb[:, 0:NT]), r=[PBB], w=[VTOK])
                        bank = 2 + (g4 % 2)

                        def body2(g4=g4, bank=bank):
                            ins = None
                            for i in range(4):
                                g = g4 * 4 + i
                                ins = nc.tensor.matmul(ps[:, bank, i * 128:(i + 1) * 128], vtok[:, i * 128:(i + 1) * 128], wsS[:, g * 128:(g + 1) * 128],
                                                       start=True, stop=True)
                            return ins
                        P(body2, r=[VTOK, WSS], w=[PB[bank]])
                        V(lambda g4=g4, bank=bank: nc.vector.tensor_tensor(out=svt[:], in0=ps[:, bank, :], in1=bias_bc[:, g4 * 512:(g4 + 1) * 512], op=ALU.add),
                          r=[PB[bank], HT], w=[SVT])
                        ug = uT[:, g4 * 4:(g4 + 1) * 4, n * 128:(n + 1) * 128]
                        V(lambda ug=ug: nc.vector.tensor_tensor(out=ug, in0=svt[:, :].rearrange("p (a b) -> p a b", a=4), in1=ug, op=ALU.mult),
                          r=[SVT] + UB[g4 * 4:(g4 + 1) * 4], w=UB[g4 * 4:(g4 + 1) * 4])
                k.fence()
                stm = Stream(tiles, RING[:3], srcs4(wout, 0, 0, KC))
                proj4(stm, 0, KC, lambda kc: uT[:, kc, :], UB, resid_epilogue(2, 1), [1, 2, 3, 4])
                k.fence()
            with ExitStack() as ph:
                gtR = sb(ph, "gtR", [1, NT], F32)
                gts = sb(ph, "gts", [128, 4, NE], F32)
                gb = [sb(ph, "gb%d" % i, [128, NT], F32) for i in range(2)]
                GTR = Buf("gtR")
                GTS = Buf("gts")
                GB = [Buf("gb0"), Buf("gb1")]
                with ExitStack() as ph2:
                    rt = [sb(ph2, "rt%d" % i, [128, NT], F32) for i in range(2)]
                    RT = [Buf("rt0"), Buf("rt1")]
                    norm_mod(3, router=True, rt=rt, RT=RT)
                    lgT = sb(ph2, "lgT", [NE, NT], F32)
                    lg = sb(ph2, "lg", [128, 4, NE], F32)
                    sm = sb(ph2, "sm", [128, 16], F32)
                    w8 = [sb(ph2, "w8%d" % i, [128, NE], F32) for i in range(3)]
                    LGT, LG, SM = Buf("lgT"), Buf("lg"), Buf("sm")
                    W8 = [Buf("w8%d" % i) for i in range(3)]
                    A(lambda: nc.scalar.copy(out=lgT[:], in_=ps[0:NE, 1, :]), r=[PB[1]], w=[LGT])

                    def body():
                        ins = None
                        for t in range(4):
                            ins = nc.tensor.transpose(ps[:, 2, t * NE:(t + 1) * NE], lgT[:, t * 128:(t + 1) * 128], ident[0:NE, 0:NE])
                        return ins
                    P(body, r=[LGT, CONST], w=[PB[2]])
                    V(lambda: nc.vector.tensor_copy(out=lg[:, :, :], in_=ps[:, 2, 0:4 * NE].rearrange("p (a b) -> p a b", a=4)), r=[PB[2]], w=[LG])
                    for t in range(4):
                        L = lg[:, t, :]
                        m1, nm1, m2, ssum, rs = (sm[:, i:i + 1] for i in range(5))
                        V(lambda L=L: nc.vector.reduce_max(out=m1, in_=L, axis=AX.X), r=[LG], w=[SM])
                        V(lambda L=L: nc.vector.tensor_scalar(out=w8[0][:], in0=L, scalar1=m1, scalar2=None, op0=ALU.is_equal), r=[LG, SM], w=[W8[0]])
                        V(lambda L=L: nc.vector.scalar_tensor_tensor(out=w8[1][:], in0=w8[0][:], scalar=-1e30, in1=L, op0=ALU.mult, op1=ALU.add),
                          r=[W8[0], LG], w=[W8[1]])
                        V(lambda: nc.vector.reduce_max(out=m2, in_=w8[1][:], axis=AX.X), r=[W8[1], SM], w=[SM])
                        V(lambda L=L: nc.vector.tensor_scalar(out=w8[0][:], in0=L, scalar1=m2, scalar2=None, op0=ALU.is_ge), r=[LG, SM], w=[W8[0]])
                        V(lambda: nc.vector.tensor_scalar(out=nm1, in0=m1, scalar1=-1.0, scalar2=None, op0=ALU.mult), r=[SM], w=[SM])
                        A(lambda L=L: nc.scalar.activation(out=w8[2][:], in_=L, func=AF.Exp, bias=nm1, scale=1.0), r=[LG, SM], w=[W8[2]])
                        V(lambda: nc.vector.tensor_tensor(out=w8[1][:], in0=w8[2][:], in1=w8[0][:], op=ALU.mult), r=[W8[2], W8[0]], w=[W8[1]])
                        V(lambda: nc.vector.reduce_sum(out=ssum, in_=w8[1][:], axis=AX.X), r=[W8[1], SM], w=[SM])
                        V(lambda: nc.vector.reciprocal(out=rs, in_=ssum), r=[SM], w=[SM])
                        V(lambda t=t: nc.vector.tensor_scalar(out=gts[:, t, :], in0=w8[1][:], scalar1=rs, scalar2=None, op0=ALU.mult), r=[W8[1], SM], w=[GTS])
                    k.fence()

                def mk_pre(e):
                    def pre():
                        def body3():
                            ins = None
                            for t in range(4):
                                ins = nc.tensor.transpose(ps[0:1, 4, t * 128:(t + 1) * 128], gts[:, t, e:e + 1], ident[:])
                            return ins
                        P(body3, r=[GTS, CONST], w=[PB[4]])
                        A(lambda: nc.scalar.copy(out=gtR[0:1, :], in_=ps[0:1, 4, :]), r=[PB[4]], w=[GTR])
                        P(lambda: nc.tensor.matmul(ps[:, 4, :], ones[0:1, :], gtR[0:1, :], start=True, stop=True),
                          r=[CMISC, GTR], w=[PB[4]])
                        A(lambda: nc.scalar.copy(out=gb[e % 2][:], in_=ps[:, 4, :]), r=[PB[4]], w=[GB[e % 2]])
                    return pre
                groups = []
                for e in range(NE):
                    for g in range(DE // 256):
                        groups.append((wsrc(mg, e * D, g * WT), wsrc(mu, e * D, g * WT), dsrc(md, e * DE + g * 256), gb[e % 2][:], [GB[e % 2]],
                                       mk_pre(e) if g == 0 else None))
                glu(groups, 1, ph)
                k.fence()
            compute_rstd(1.0 / D)
            with ExitStack() as ph:
                yst = sb(ph, "yst", [128, 4, D], F32)
                YS = Buf("yst")
                for kc in range(KC):
                    t = 2 + (kc % 2)
                    V(lambda kc=kc, t=t: nc.vector.scalar_tensor_tensor(out=tmpf[t][:], in0=xT[:, kc, :], scalar=gT[:, 4 * KC + kc:4 * KC + kc + 1],
                                                                        in1=rstd[:], op0=ALU.mult, op1=ALU.mult),
                      r=[XB[kc], RSTD, CONST], w=[TB[t]])
                    bank = 2 + (kc % 4)

                    def body(t=t, bank=bank):
                        ins = None
                        for tt in range(4):
                            ins = nc.tensor.transpose(ps[:, bank, tt * 128:(tt + 1) * 128], tmpf[t][:, tt * 128:(tt + 1) * 128], ident[:])
                        return ins
                    P(body, r=[TB[t], CONST], w=[PB[bank]])
                    src = ps[:, bank, :].rearrange("p (a b) -> p a b", a=4)
                    dst = yst[:, :, kc * 128:(kc + 1) * 128]
                    if kc % 2 == 0:
                        A(lambda dst=dst, src=src: nc.scalar.copy(out=dst, in_=src), r=[PB[bank]], w=[YS])
                    else:
                        V(lambda dst=dst, src=src: nc.vector.tensor_copy(out=dst, in_=src), r=[PB[bank]], w=[YS])
                for tt in range(4):
                    k.dma(k.sp, y[tok0 + tt * 128: tok0 + (tt + 1) * 128, :], yst[:, tt, :], reads=[YS], writes=[YB])
                k.fence()
        k.finish(k.sp)
    return nc


def rope_tables(S):
    pos = np.arange(S)
    row = (pos // 64).astype(np.float32)
    col = (pos % 64).astype(np.float32)
    inv = (1.0 / (np.float32(10000.0) ** (np.arange(32, dtype=np.float32) * np.float32(2.0) / np.float32(64)))).astype(np.float32)
    ang = np.zeros((128, S), np.float32)
    for d in range(128):
        f = inv[d % 32]
        ang[d] = (row if d < 64 else col) * f
    return np.cos(ang).astype(np.float32), np.sin(ang).astype(np.float32)


def rot_matrix():
    R = np.zeros((128, 128), np.float32)
    for half in (0, 64):
        for i in range(32):
            R[half + 32 + i, half + i] = -1.0
            R[half + i, half + 32 + i] = 1.0
    return R


def chunkT(v, kc):
    return np.ascontiguousarray(np.asarray(v, np.float32).reshape(kc, 128).T)


def make_in_maps(cfg, inputs, n_cores):
    D, S, NOWN, DFF, NE, DE = cfg.D, cfg.S, cfg.NOWN, cfg.DFF, cfg.NE, cfg.DE
    KC = D // 128
    g = lambda n: np.asarray(inputs[n], np.float32)
    cs, sn = rope_tables(S)
    per_b = S // NOWN
    shared = {
        "wmod": np.ascontiguousarray(g("w_mod").reshape(2 * D, 6 * D)),
        "bmodT": np.concatenate([chunkT(g("b_mod")[i], 6 * KC) for i in range(2)], axis=1),
        "gT": np.concatenate([chunkT(g("norm_g")[0, 0], KC), chunkT(g("norm_g")[0, 1], KC), chunkT(g("norm_g")[1, 0], KC),
                              chunkT(g("norm_g")[1, 1], KC), chunkT(g("final_g"), KC)], axis=1),
        "wqkv": g("attn_w_qkv")[0],
        "qkg": np.ascontiguousarray(np.stack([g("attn_q_gain")[0], g("attn_k_gain")[0]], axis=1)),
        "wo": g("attn_w_o")[0],
        "wuv": g("mix_w_uv")[0],
        "vgT": chunkT(g("mix_v_gain")[0], KC),
        "wsT": np.ascontiguousarray(g("mix_w_s")[0].transpose(2, 0, 1).reshape(128, KC * 128)),
        "bsb": np.ascontiguousarray(np.broadcast_to(g("mix_b_s")[0].reshape(1, KC * 128), (128, KC * 128))),
        "wout": g("mix_w_out")[0],
        "wg": g("ffn_w_gate")[0], "wu": g("ffn_w_up")[0], "wd": g("ffn_w_down")[0],
        "wr": np.ascontiguousarray(g("moe_w_router")[0].reshape(KC, 128, NE).transpose(1, 0, 2).reshape(128, KC * NE)),
        "mg": g("moe_w_gate")[0].reshape(NE * D, DE), "mu": g("moe_w_up")[0].reshape(NE * D, DE),
        "md": g("moe_w_down")[0].reshape(NE * DE, D),
        "ident": np.eye(128, dtype=np.float32), "rot": rot_matrix(),
        "cs": cs, "sn": sn,
    }
    maps = []
    x = g("x")
    c = g("c")
    for core in range(n_cores):
        b, q = core // per_b, core % per_b
        m = dict(shared)
        m["xb"] = x[b]
        m["xo"] = x[b, q * NOWN:(q + 1) * NOWN]
        m["cso"] = np.ascontiguousarray(cs[:, q * NOWN:(q + 1) * NOWN])
        m["sno"] = np.ascontiguousarray(sn[:, q * NOWN:(q + 1) * NOWN])
        m["cT"] = chunkT(c[b], KC)
        maps.append(m)
    return maps


def kernel(**inputs):
    cfg = FULL
    nc = build(cfg)
    maps = make_in_maps(cfg, inputs, 8)
    res = run_bass_kernel_spmd(nc, maps, core_ids=list(range(8)))
    out = np.zeros((2, cfg.S, cfg.D), np.float32)
    per_b = cfg.S // cfg.NOWN
    for core in range(8):
        b, q = core // per_b, core % per_b
        out[b, q * cfg.NOWN:(q + 1) * cfg.NOWN] = res.results[core]["y"]
    return out
```
